# Optimizing a Trainium2 kernel written in Bass

```python
import jax, jax.numpy as jnp
from jax import lax
import numpy as np

D_MODEL = 2048
BATCH = 2
SEQ = 4096
DEPTH = 4
DEC_BATCH = 32
DEC_SEQ = 4
PAST_LEN = 16384
PAGE_SIZE = 128

RET_DK = 128
RET_DV = 128
RET_HEADS = (D_MODEL // 2) // RET_DV
RET_CHUNK = 128
ATT_HEAD_DIM = 64
ATT_Q_HEADS = (D_MODEL // 4) // ATT_HEAD_DIM
ATT_KV_HEADS = 2
ATT_GROUP = ATT_Q_HEADS // ATT_KV_HEADS
WINDOW = 128
CONV_WIDTH = 3
CONV_CH = D_MODEL - RET_HEADS * RET_DV - ATT_Q_HEADS * ATT_HEAD_DIM
D_FF = -(-8 * D_MODEL // (3 * 256)) * 256
N_MOD = 6
EPS = 1e-6
NEG_INF = -1e30
SPLIT_SIZES = (RET_HEADS * RET_DK, RET_HEADS * RET_DK, RET_HEADS * RET_DV, RET_HEADS * RET_DV,
               ATT_Q_HEADS * ATT_HEAD_DIM, ATT_KV_HEADS * ATT_HEAD_DIM, ATT_KV_HEADS * ATT_HEAD_DIM,
               CONV_CH, CONV_CH, CONV_CH)
PROJ_WIDTH = sum(SPLIT_SIZES)

kernel_name = 'hybrid_retention_swa_shortconv_step'


def _split_points():
    pts, acc = [], 0
    for s in SPLIT_SIZES[:-1]:
        acc += s
        pts.append(acc)
    return pts


def _rmsnorm(x, g):
    xf = x.astype(jnp.float32)
    y = xf * lax.rsqrt(jnp.mean(xf * xf, axis=-1, keepdims=True) + EPS)
    return (y * g.astype(jnp.float32)).astype(x.dtype)


def _ret_log_gamma():
    return jnp.log1p(-jnp.exp2(-5.0 - jnp.arange(RET_HEADS, dtype=jnp.float32)))


def _alibi_slopes():
    h = jnp.arange(1, ATT_Q_HEADS + 1, dtype=jnp.float32)
    return jnp.exp2(-8.0 * h / ATT_Q_HEADS).reshape(ATT_KV_HEADS, ATT_GROUP)


def _retention_chunk(q, k, v, s0, log_gamma):
    t = q.shape[1]
    pos = jnp.arange(t, dtype=jnp.float32)
    diff = pos[:, None] - pos[None, :]
    decay = jnp.where(diff[None] >= 0,
                      jnp.exp(jnp.maximum(diff, 0.0)[None] * log_gamma[:, None, None]), 0.0)
    scores = jnp.einsum('bihd,bjhd->bhij', q, k) * decay[None]
    intra = jnp.einsum('bhij,bjhe->bihe', scores, v)
    q_decay = jnp.exp((pos[:, None] + 1.0) * log_gamma[None, :])
    inter = jnp.einsum('bihd,bhde->bihe', q, s0) * q_decay[None, :, :, None]
    k_decay = jnp.exp((t - 1.0 - pos)[:, None] * log_gamma[None, :])
    s_new = (jnp.exp(t * log_gamma)[None, :, None, None] * s0
             + jnp.einsum('bjhd,bjhe->bhde', k * k_decay[None, :, :, None], v))
    return intra + inter, s_new


def _retention_prompt(q, k, v, log_gamma):
    b, s = q.shape[:2]
    nc = s // RET_CHUNK

    def to_chunks(a):
        return a.reshape((b, nc, RET_CHUNK) + a.shape[2:]).swapaxes(0, 1)

    def step(state, qkv):
        qc, kc, vc = qkv
        y, state = _retention_chunk(qc, kc, vc, state, log_gamma)
        return state, y

    s0 = jnp.zeros((b, RET_HEADS, RET_DK, RET_DV), jnp.float32)
    s_fin, ys = lax.scan(step, s0, (to_chunks(q), to_chunks(k), to_chunks(v)))
    return ys.swapaxes(0, 1).reshape(b, s, RET_HEADS, RET_DV), s_fin


def _sink_attention(q, k, v, q_pos, k_pos, sinks, slopes):
    s = jnp.einsum('bnqhgd,bnkhd->bnhgqk', q, k) * (ATT_HEAD_DIM ** -0.5)
    dist = q_pos[:, :, None] - k_pos[:, None, :]
    allowed = (dist >= 0) & (dist < WINDOW) & (k_pos[:, None, :] >= 0)
    s = s - slopes[None, None, :, :, None, None] * dist.astype(jnp.float32)[None, :, None, None]
    s = jnp.where(allowed[None, :, None, None], s, NEG_INF)
    sink = jnp.broadcast_to(sinks.reshape(ATT_KV_HEADS, ATT_GROUP)[None, None, :, :, None, None],
                            s.shape[:-1] + (1,))
    p = jax.nn.softmax(jnp.concatenate([s, sink], axis=-1), axis=-1)[..., :-1]
    return jnp.einsum('bnhgqk,bnkhd->bnqhgd', p, v)


def _swa_prompt(q, k, v, sinks, slopes):
    b, s = q.shape[:2]
    nb = s // WINDOW
    qb = q.reshape(b, nb, WINDOW, ATT_KV_HEADS, ATT_GROUP, ATT_HEAD_DIM)

    def band(a):
        ap = jnp.pad(a, ((0, 0), (WINDOW, 0), (0, 0), (0, 0)))
        ap = ap.reshape(b, nb + 1, WINDOW, ATT_KV_HEADS, ATT_HEAD_DIM)
        return jnp.concatenate([ap[:, :-1], ap[:, 1:]], axis=2)

    q_pos = jnp.arange(s, dtype=jnp.int32).reshape(nb, WINDOW)
    kp = jnp.arange(-WINDOW, s, dtype=jnp.int32).reshape(nb + 1, WINDOW)
    k_pos = jnp.concatenate([kp[:-1], kp[1:]], axis=1)
    o = _sink_attention(qb, band(k), band(v), q_pos, k_pos, sinks, slopes)
    return o.reshape(b, s, ATT_Q_HEADS * ATT_HEAD_DIM)


def _swa_step(q, k, v, buf_k, buf_v, sinks, slopes):
    b, t = q.shape[:2]
    w = buf_k.shape[1]
    k_all = jnp.concatenate([buf_k, k], axis=1)
    v_all = jnp.concatenate([buf_v, v], axis=1)
    q_pos = (PAST_LEN + jnp.arange(t, dtype=jnp.int32))[None]
    k_pos = jnp.concatenate([PAST_LEN - w + jnp.arange(w, dtype=jnp.int32),
                             PAST_LEN + jnp.arange(t, dtype=jnp.int32)])[None]
    o = _sink_attention(q[:, None], k_all[:, None], v_all[:, None], q_pos, k_pos, sinks, slopes)
    return o.reshape(b, t, ATT_Q_HEADS * ATT_HEAD_DIM), k_all[:, -w:], v_all[:, -w:]


def _short_conv(gate_b, gate_c, hx, buf, conv_w):
    u = gate_c * hx
    up = jnp.concatenate([buf, u], axis=1)
    t = u.shape[1]
    y = conv_w[0] * up[:, 0:t]
    for j in range(1, CONV_WIDTH):
        y = y + conv_w[j] * up[:, j:j + t]
    return gate_b * y, up[:, -(CONV_WIDTH - 1):]


def _mixers(h, w_in_l, w_out_l, conv_w_l, sinks_l, s_ret, buf_k, buf_v, buf_conv, w_buf, sdt, is_prompt):
    f32 = jnp.float32
    b, t, _ = h.shape
    proj = (h @ w_in_l).astype(f32)
    rq, rk, rv, rg, aq, ak, av, cb, cc, ch = jnp.split(proj, _split_points(), axis=-1)
    rq = rq.reshape(b, t, RET_HEADS, RET_DK)
    rk = rk.reshape(b, t, RET_HEADS, RET_DK) * (RET_DK ** -0.5)
    rv = rv.reshape(b, t, RET_HEADS, RET_DV)
    log_gamma = _ret_log_gamma()
    if is_prompt:
        ry, s_new = _retention_prompt(rq, rk, rv, log_gamma)
    else:
        ry, s_new = _retention_chunk(rq, rk, rv, s_ret.astype(f32), log_gamma)
    ry = ry * lax.rsqrt(jnp.mean(ry * ry, axis=-1, keepdims=True) + EPS)
    ret_out = jax.nn.silu(rg) * ry.reshape(b, t, RET_HEADS * RET_DV)
    aq = aq.reshape(b, t, ATT_KV_HEADS, ATT_GROUP, ATT_HEAD_DIM)
    ak = ak.reshape(b, t, ATT_KV_HEADS, ATT_HEAD_DIM)
    av = av.reshape(b, t, ATT_KV_HEADS, ATT_HEAD_DIM)
    sinks = sinks_l.astype(f32)
    slopes = _alibi_slopes()
    if is_prompt:
        att_out = _swa_prompt(aq, ak, av, sinks, slopes)
        k_new, v_new = ak[:, -w_buf:], av[:, -w_buf:]
    else:
        att_out, k_new, v_new = _swa_step(aq, ak, av, buf_k.astype(f32), buf_v.astype(f32), sinks, slopes)
    conv_out, conv_new = _short_conv(cb, cc, ch, buf_conv.astype(f32), conv_w_l.astype(f32))
    mixed = jnp.concatenate([ret_out, att_out, conv_out], axis=-1).astype(h.dtype)
    out = mixed @ w_out_l
    return out, (s_new.astype(sdt), k_new.astype(sdt), v_new.astype(sdt), conv_new.astype(sdt))


def _layer(x, c, mix, norm_g_l, w_ada_l, b_ada_l, w_gate, w_up, w_down):
    mod = jax.nn.silu(c) @ w_ada_l + b_ada_l
    sh1, sc1, g1, sh2, sc2, g2 = jnp.split(mod[:, None, :], N_MOD, axis=-1)
    h = _rmsnorm(x, norm_g_l[0]) * (1.0 + sc1) + sh1
    o, new_state = mix(h)
    x = x + g1 * _rmsnorm(o, norm_g_l[1])
    h = _rmsnorm(x, norm_g_l[2]) * (1.0 + sc2) + sh2
    f = (jax.nn.silu(h @ w_gate) * (h @ w_up)) @ w_down
    x = x + g2 * _rmsnorm(f, norm_g_l[3])
    return x, new_state


def setup_inputs(seed: int = 0) -> dict:
    key = jax.random.key(seed)
    ks = jax.random.split(key, 20)
    f32 = jnp.float32
    w_buf = min(WINDOW, PAST_LEN)

    def nrm(k, shape, scale):
        return jax.random.normal(k, shape, f32) * scale

    return {
        'x_prompt': nrm(ks[0], (BATCH, SEQ, D_MODEL), 1.0),
        'x_sample': nrm(ks[1], (DEC_BATCH, DEC_SEQ, D_MODEL), 1.0),
        'state_ret': nrm(ks[2], (DEPTH, DEC_BATCH, RET_HEADS, RET_DK, RET_DV), 1.0),
        'cache_win_k': nrm(ks[3], (DEPTH, DEC_BATCH, w_buf, ATT_KV_HEADS, ATT_HEAD_DIM), 1.0),
        'cache_win_v': nrm(ks[4], (DEPTH, DEC_BATCH, w_buf, ATT_KV_HEADS, ATT_HEAD_DIM), 1.0),
        'state_conv': nrm(ks[5], (DEPTH, DEC_BATCH, CONV_WIDTH - 1, CONV_CH), 1.0),
        'c_prompt': nrm(ks[6], (BATCH, D_MODEL), 1.0),
        'c_sample': nrm(ks[7], (DEC_BATCH, D_MODEL), 1.0),
        'w_in': nrm(ks[8], (DEPTH, D_MODEL, PROJ_WIDTH), D_MODEL ** -0.5),
        'w_out': nrm(ks[9], (DEPTH, D_MODEL, D_MODEL), D_MODEL ** -0.5),
        'conv_w': nrm(ks[10], (DEPTH, CONV_WIDTH, CONV_CH), CONV_WIDTH ** -0.5),
        'attn_sinks': nrm(ks[11], (DEPTH, ATT_Q_HEADS), 1.0),
        'norm_g': 1.0 + nrm(ks[12], (DEPTH, 4, D_MODEL), 0.05),
        'w_ada': nrm(ks[13], (DEPTH, D_MODEL, N_MOD * D_MODEL), 0.5 * D_MODEL ** -0.5),
        'b_ada': nrm(ks[14], (DEPTH, N_MOD * D_MODEL), 0.01),
        'w_ff_gate': nrm(ks[15], (DEPTH, D_MODEL, D_FF), D_MODEL ** -0.5),
        'w_ff_up': nrm(ks[16], (DEPTH, D_MODEL, D_FF), D_MODEL ** -0.5),
        'w_ff_down': nrm(ks[17], (DEPTH, D_FF, D_MODEL), D_FF ** -0.5),
    }


def reference(x_prompt, x_sample, state_ret, cache_win_k, cache_win_v, state_conv, c_prompt, c_sample,
              w_in, w_out, conv_w, attn_sinks, norm_g, w_ada, b_ada, w_ff_gate, w_ff_up, w_ff_down):
    w_buf = cache_win_k.shape[2]
    sdt = state_ret.dtype
    zeros_conv = jnp.zeros((x_prompt.shape[0], CONV_WIDTH - 1, CONV_CH), x_prompt.dtype)
    y_p, y_s = x_prompt, x_sample
    ret_p, ret_s, wk_p, wk_s, wv_p, wv_s, cv_p, cv_s = [], [], [], [], [], [], [], []
    for l in range(DEPTH):
        ffn = (norm_g[l], w_ada[l], b_ada[l], w_ff_gate[l], w_ff_up[l], w_ff_down[l])
        mix_p = lambda h: _mixers(h, w_in[l], w_out[l], conv_w[l], attn_sinks[l], None, None, None,
                                  zeros_conv, w_buf, sdt, True)
        mix_s = lambda h: _mixers(h, w_in[l], w_out[l], conv_w[l], attn_sinks[l], state_ret[l],
                                  cache_win_k[l], cache_win_v[l], state_conv[l], w_buf, sdt, False)
        y_p, (sr_p, k_p, v_p, c_p) = _layer(y_p, c_prompt, mix_p, *ffn)
        y_s, (sr_s, k_s, v_s, c_s) = _layer(y_s, c_sample, mix_s, *ffn)
        ret_p.append(sr_p); ret_s.append(sr_s)
        wk_p.append(k_p); wk_s.append(k_s)
        wv_p.append(v_p); wv_s.append(v_s)
        cv_p.append(c_p); cv_s.append(c_s)
    return (y_p, y_s, jnp.stack(ret_p), jnp.stack(ret_s), jnp.stack(wk_p), jnp.stack(wk_s),
            jnp.stack(wv_p), jnp.stack(wv_s), jnp.stack(cv_p), jnp.stack(cv_s))
```

```python
import numpy as np
import ml_dtypes
import concourse.bass as bass
import concourse.mybir as mybir
from concourse.bass_utils import run_bass_kernel_spmd

F32 = mybir.dt.float32
BF16 = mybir.dt.bfloat16
AF = mybir.ActivationFunctionType
ALU = mybir.AluOpType
AX = mybir.AxisListType
EPS = 1e-6
NEG = -1e30
NCORE = 8


class Cfg:
    def __init__(s, D=2048, SEQ=4096, DEPTH=4, DEC_BATCH=32, TS=4, BATCH=2):
        s.D = D
        s.KC = D // 128
        s.RH = (D // 2) // 128
        s.AQ = (D // 4) // 64
        s.GRP = s.AQ // 2
        s.CC = D - s.RH * 128 - s.AQ * 64
        s.CCH = s.CC // 128
        s.DFF = -(-8 * D // (3 * 256)) * 256
        s.FC = s.DFF // 128
        s.BATCH = BATCH
        s.SEQ = SEQ
        s.SEGS = NCORE // BATCH
        s.NP = SEQ // s.SEGS
        s.NCH = s.NP // 128
        s.DEC_BATCH = DEC_BATCH
        s.NSQ = DEC_BATCH // NCORE
        s.TS = TS
        s.NS = s.NSQ * TS
        s.NT = s.NP + s.NS
        s.DEPTH = DEPTH
        s.NQ = 1 + s.NSQ
        gs = 8 if s.FC % 8 == 0 or s.FC > 16 else 2
        s.GROUPS = []
        r = s.FC
        while r > 0:
            s.GROUPS.append(min(gs, r))
            r -= min(gs, r)
        s.GMAX = max(s.GROUPS)
        s.BL = []
        for h in range(s.RH):
            s.BL += [("rk", h), ("rv", h)]
        s.BL += [("akd", 0), ("akd", 1), ("av", 0)]
        for c in range(s.CCH):
            s.BL += [("cb", c), ("cc", c), ("ch", c)]
        for h in range(s.RH):
            s.BL += [("rq", h), ("rk", h), ("rv", h), ("rg", h)]
        for c in range(s.AQ // 2):
            s.BL += [("aq", c)]
        s.NB_IN = len(s.BL)
        s.TT = []
        c0 = 0
        while c0 < s.NP:
            w = min(512, s.NP - c0)
            s.TT.append((c0, w))
            c0 += w
        s.TT.append((s.NP, s.NS))
        s.SPL = s.TT[0][1]
        o = 0
        s.c_decT = o; o += s.RH * 128
        s.c_qdec = o; o += s.RH * 128
        s.c_kdP = o; o += s.RH
        s.c_kdS = o; o += s.RH
        s.c_ab = o; o += s.AQ * 256
        s.c_coef = o; o += NCORE * s.RH
        s.c_oh = o; o += NCORE
        s.c_negf = o; o += 1
        s.c_eps = o; o += 1
        s.NCST = o
        o = 0
        s.l_ng = o; o += 4 * s.KC
        s.l_ba = o; o += 6 * s.KC
        s.l_cw = o; o += 3 * s.CCH
        s.l_sk = o; o += s.AQ
        s.NLP = o
        o = 0
        s.b_S = o; o += s.RH * 128
        s.b_k = o; o += 256
        s.b_v = o; o += 128
        s.b_u = o; o += s.CCH * 2
        s.NBC = o
        s.gam = [1.0 - 2.0 ** (-5 - h) for h in range(s.RH)]
        s.slopes = [2.0 ** (-8.0 * (h + 1) / s.AQ) for h in range(s.AQ)]

    def wcol(s, kind, i):
        D = s.D
        R = s.RH * 128
        A = s.AQ * 64
        base = {"rq": 0, "rk": R, "rv": 2 * R, "rg": 3 * R, "aq": 4 * R, "ak": 4 * R + A,
                "av": 4 * R + A + 128, "cb": 4 * R + A + 256, "cc": 4 * R + A + 256 + s.CC,
                "ch": 4 * R + A + 256 + 2 * s.CC}
        if kind == "akd":
            c = np.arange(64) + base["ak"] + 64 * i
            return np.concatenate([c, c])
        return np.arange(128) + base[kind] + 128 * i


class Sched:
    ENG = ("pe", "act", "dve", "pool", "sp")

    def __init__(s):
        s.ops = []
        s.lw = {}
        s.rd = {}

    def add(s, eng, fn, r=(), w=(), kind="c"):
        idx = len(s.ops)
        w = list(w) + [k for k in r if isinstance(k, tuple) and k[0] == "PB" and k not in w]
        deps = set()
        for k in r:
            x = s.lw.get(k)
            if x is not None:
                deps.add(x)
        for k in w:
            x = s.lw.get(k)
            if x is not None:
                deps.add(x)
            for x in s.rd.get(k, ()):
                o = s.ops[x]
                deps.add(x)
        fdeps = []
        for x in deps:
            o = s.ops[x]
            if o["kind"] == "c" and kind == "c" and eng == "pe" and o["eng"] == "pe":
                continue
            fdeps.append(x)
        s.ops.append(dict(eng=eng, fn=fn, deps=fdeps, kind=kind, sig=False))
        for k in r:
            s.rd.setdefault(k, []).append(idx)
        for k in w:
            s.lw[k] = idx
            s.rd[k] = []
        return idx

    def emit(s, nc, block, esem, dsems, ccsem):
        NDS = len(dsems["pool"])
        for o in s.ops:
            for x in o["deps"]:
                s.ops[x]["sig"] = True
        cnt = {e: 0 for e in s.ENG}
        dcount = {q: 0 for q in ("pool", "sp")}
        dtarget = {q: [0] * NDS for q in ("pool", "sp")}
        ncc = 0
        for o in s.ops:
            e = o["eng"]
            if o["kind"] == "c":
                if o["sig"]:
                    cnt[e] += 1
                    o["ev"] = (("e", e), cnt[e])
            elif o["kind"] == "d":
                i = dcount[e] % NDS
                dcount[e] += 1
                o["prev"] = (("d", e, i), dtarget[e][i])
                dtarget[e][i] += 16
                o["ev"] = (("d", e, i), dtarget[e][i])
            else:
                ncc += 1
                o["ev"] = (("cc",), ncc)

        def semof(k):
            if k[0] == "e":
                return esem[k[1]]
            if k[0] == "d":
                return dsems[k[1]][k[2]]
            return ccsem

        per = {e: [] for e in s.ENG}
        for o in s.ops:
            per[o["eng"]].append(o)
        handles = {"pe": nc.tensor, "act": nc.scalar, "dve": nc.vector, "pool": nc.gpsimd, "sp": nc.sync}
        final = []
        for q in ("pool", "sp"):
            for i in range(NDS):
                if dtarget[q][i] > 0:
                    final.append((("d", q, i), dtarget[q][i]))

        def run(e, eng):
            known = {}
            for o in per[e]:
                waits = {}
                for x in o["deps"]:
                    k, v = s.ops[x]["ev"]
                    if waits.get(k, 0) < v:
                        waits[k] = v
                if o["kind"] == "d":
                    k, v = o["prev"]
                    if v > 0 and waits.get(k, 0) < v:
                        waits[k] = v
                for k, v in waits.items():
                    if known.get(k, 0) >= v:
                        continue
                    eng.wait_ge(semof(k), v)
                    known[k] = v
                ins = o["fn"](eng)
                if o["kind"] == "c":
                    if o["sig"]:
                        ins.then_inc(esem[e], 1)
                elif o["kind"] == "d":
                    ins.then_inc(semof(o["ev"][0]), 16)
                else:
                    ins.then_inc(ccsem)
            if e == "sp":
                for k, v in final:
                    eng.wait_ge(semof(k), v)
                for ee in ("pe", "act", "dve"):
                    if cnt[ee] > 0:
                        eng.wait_ge(esem[ee], cnt[ee])

        @block.tensor
        def _(eng):
            run("pe", eng)

        @block.scalar
        def _(eng):
            run("act", eng)

        @block.vector
        def _(eng):
            run("dve", eng)

        @block.gpsimd
        def _(eng):
            run("pool", eng)

        @block.sync
        def _(eng):
            run("sp", eng)


def build(cfg):
    C = cfg
    nc = bass.Bass("TRN2", target_bir_lowering=False)
    KC, RH, AQ, GRP, CCH, NP, NS, NT, NCH, NSQ, TS, NQ, FC = (
        C.KC, C.RH, C.AQ, C.GRP, C.CCH, C.NP, C.NS, C.NT, C.NCH, C.NSQ, C.TS, C.NQ, C.FC)
    DEPTH = C.DEPTH
    WN = KC * 128

    def din(name, shape):
        return nc.dram_tensor(name, list(shape), F32, kind="ExternalInput").ap()

    def dout(name, shape):
        return nc.dram_tensor(name, list(shape), F32, kind="ExternalOutput").ap()

    xT = din("xT", [C.D, NT])
    cT = din("cT", [C.D, NQ])
    w_in = din("w_in", [DEPTH, C.NB_IN, 128, WN])
    w_out = din("w_out", [DEPTH, KC, 128, WN])
    w_ada = din("w_ada", [DEPTH, 6 * KC, 128, WN])
    w_gate = din("w_gate", [DEPTH, FC, 128, WN])
    w_up = din("w_up", [DEPTH, FC, 128, WN])
    w_down = din("w_down", [DEPTH, len(C.GROUPS), KC, 128, C.GMAX * 128])
    lp_in = din("lp", [128, DEPTH, C.NLP])
    cst_in = din("cst", [128, C.NCST])
    idn_in = din("idn", [128, 128])
    st_ret = din("st_ret", [DEPTH, NSQ, RH, 128, 128])
    c_kd = din("c_kd", [DEPTH, NSQ, 2, 128, 128])
    c_v = din("c_v", [DEPTH, NSQ, 128, 128])
    c_vT = din("c_vT", [DEPTH, NSQ, 128, 128])
    st_cv = din("st_cv", [DEPTH, 128, CCH, NSQ, 2])

    yT = dout("yT", [C.D, NT])
    o_retp = dout("o_retp", [DEPTH, RH, 128, 128])
    o_rets = dout("o_rets", [DEPTH, NSQ, RH, 128, 128])
    o_wkp = dout("o_wkp", [DEPTH, 2, 64, 128])
    o_wks = dout("o_wks", [DEPTH, NSQ, 2, 64, 128])
    o_wvp = dout("o_wvp", [DEPTH, 128, 128])
    o_wvs = dout("o_wvs", [DEPTH, NSQ, 128, 128])
    o_cvp = dout("o_cvp", [DEPTH, 128, CCH, 2])
    o_cvs = dout("o_cvs", [DEPTH, 128, CCH, NSQ, 2])

    bounce = [nc.dram_tensor(f"bounce{l}", [128, C.NBC], F32) for l in range(DEPTH)]
    gath = [nc.dram_tensor(f"gath{l}", [NCORE * 128, C.NBC], F32) for l in range(DEPTH)]

    S = Sched()
    A = {}
    import os
    KSTOP = int(os.environ.get("K_STOP", "99"))

    class StopBuild(Exception):
        pass

    def chk(n):
        if KSTOP == n:
            raise StopBuild()

    def sb(name, shape, dt):
        A[name] = nc.alloc_sbuf_tensor("s_" + name, list(shape), dt).ap()
        return A[name]

    NSLOT = 3
    HBW = max(KC * NT * 2, KC * (NT - C.SPL) * 4) // 4
    xd = nc.dram_tensor("xd", [KC, 128, NT], F32)
    XC = sb("XC", [128, 2, NT], F32)
    HBf = sb("HB", [128, HBW], F32)
    SCR1W = 4 * (NT // 2) + 2 * NT + 3 * ((NCH + NSQ) * 64) + NT + NT // 2 + 16
    MFW = KC * NT // 2 + max(KC * NT // 2, SCR1W)
    MFf = sb("MF", [128, MFW], F32)
    WR = sb("WR", [128, NSLOT, WN], BF16)
    cst = sb("cst", [128, C.NCST], F32)
    lp = sb("lp", [128, DEPTH, C.NLP], F32)
    ident = sb("ident", [128, 128], BF16)
    identf = sb("identf", [128, 128], F32)
    ones = sb("ones", [128, 128], BF16)
    Hb = HBf.bitcast(BF16).rearrange("p (k n) -> p k n", k=KC)
    H = Hb[:, :, 0:NT]
    OB = HBf[:, 0:KC * (NT - C.SPL)].rearrange("p (k n) -> p k n", k=KC)
    Mb = MFf.bitcast(BF16)
    M = Mb[:, 0:KC * NT].rearrange("p (k n) -> p k n", k=KC)
    OA = MFf[:, KC * NT // 2: KC * NT // 2 + KC * C.SPL].rearrange("p (k n) -> p k n", k=KC)
    Fa = MFf[:, 0:KC * NT].rearrange("p (k n) -> p k n", k=KC)
    HIDW = max(C.GMAX * NT // 2, 4 * NT + 8, 3 * NT + GRP * 256)
    HIDf = sb("HIDf", [128, HIDW], F32)
    HID = HIDf[:, 0:C.GMAX * NT // 2].bitcast(BF16).rearrange("p (j n) -> p j n", j=C.GMAX)

    class Carver:
        def __init__(s, base, lo, hi):
            s.base, s.o, s.hi = base, lo, hi

        def f32(s, shape):
            n = int(np.prod(shape))
            assert s.o + n <= s.hi, ("scratch overflow", s.o, n, s.hi)
            v = s.base[:, s.o:s.o + n]
            s.o += n
            if len(shape) == 2:
                return v.rearrange("p (a b) -> p a b", a=shape[0])
            return v

        def bf(s, shape):
            n = int(np.prod(shape))
            n32 = (n + 1) // 2
            assert s.o + n32 <= s.hi, ("scratch overflow", s.o, n32, s.hi)
            v = s.base[:, s.o:s.o + n32].bitcast(BF16)[:, 0:n]
            s.o += n32
            if len(shape) == 2:
                return v.rearrange("p (a b) -> p a b", a=shape[0])
            return v

    c1 = Carver(MFf, KC * NT // 2, MFW)
    qT = c1.bf([NT]); qdT = c1.bf([NT]); kT = c1.bf([NT]); vT = c1.bf([NT])
    gsl = c1.f32([NT]); ry = c1.f32([NT])
    vtok = c1.bf([NCH + NSQ, 128]); kdtok = c1.bf([NCH + NSQ, 128])
    kk = c1.bf([2, NT]); vTa = c1.bf([NT]); vtoka = c1.bf([NCH + NSQ, 128])
    c2 = Carver(HIDf, 0, HIDW)
    cb32 = c2.f32([NT]); cc32 = c2.f32([NT]); cvt1 = c2.f32([NT]); Up = c2.f32([2 + NP])
    c3 = Carver(HIDf, 0, HIDW)
    qTa = c3.bf([AQ // 2, NT])
    assert c3.o <= 2 * NT, "qTa must overlay cb32/cc32 only"
    c3.o = 3 * NT
    sbt = c3.f32([GRP, 256])

    modT = sb("modT", [128, 6 * KC, NQ], F32)
    cTs = sb("cTs", [128, KC, NQ], F32)
    cTb = sb("cTb", [128, KC, NQ], BF16)
    mA = sb("mA", [128, 2, KC, NQ], F32)
    mG = sb("mG", [128, 2, KC, NQ], F32)
    mAs = sb("mAs", [128, 2, KC, NS], F32)
    mBs = sb("mBs", [128, 2, KC, NS], F32)
    mGs = sb("mGs", [128, 2, KC, NS], F32)
    Rn = sb("Rn", [128, NT], F32)
    sq = sb("sq", [128, 2, NT], BF16)
    tmpA = sb("tmpA", [128, 2, 512], F32)
    tmpS = sb("tmpS", [128, KC, NS], F32)
    abias0 = sb("abias0", [128, AQ, 128], F32)
    S32 = sb("S32", [128, 2, 128], F32)
    Sbf = sb("Sbf", [128, 2, 128], BF16)
    sTt = sb("sTt", [128, 2, 128], BF16)
    STG = sb("STG", [128, C.NBC], F32)
    Gt = sb("Gt", [128, NCORE, 128], F32)
    Gu = sb("Gu", [128, NCORE, CCH * 2], F32)
    Sst = sb("Sst", [128, RH, 128], F32)
    pK = sb("pK", [128, 2, 128], BF16)
    pV = sb("pV", [128, 128], BF16)
    ptmp = sb("ptmp", [128, 128], F32)
    utail = sb("utail", [128, CCH, 2], F32)
    s0f = sb("s0f", [128, 2, 128], F32)
    k32 = sb("k32", [128, 2, 128 + NS], F32)
    v32 = sb("v32", [128, 128 + NS], F32)
    ckk = sb("ckk", [128, 2, 2, 128], BF16)
    cvt = sb("cvt", [128, 2, 128], BF16)
    pbt = sb("pbt", [128, GRP, 256], BF16)
    pTt = sb("pTt", [128, GRP, 2, 128], BF16)
    mx = sb("mx", [128, 4, GRP], F32)
    ont = sb("ont", [128, GRP, 64], BF16)
    Us = sb("Us", [128, NSQ, 2 + TS], F32)
    cbF = sb("cbF", [128, CCH, 2], F32)
    uF = sb("uF", [128, CCH, 2], F32)
    fix = sb("fix", [128, CCH, 4], F32)

    PB = [nc.alloc_psum_tensor(f"pb{i}", [128, 1024], F32).ap() for i in range(4)]
    st = dict(bank=0, pair=0, tsl=0, msl=0, slot=0, tmpa=0, sqi=0, xc=0)
    NBK = 8

    def bank():
        i = st["bank"] % NBK
        st["bank"] += 1
        return PB[i // 2][:, (i % 2) * 512:(i % 2) * 512 + 512], [("PB", i)]

    def pair():
        if st["bank"] % 2:
            st["bank"] += 1
        i = st["bank"] % NBK
        st["bank"] += 2
        return PB[i // 2], [("PB", i), ("PB", i + 1)]

    def tslot():
        a_, k_ = bank()
        return a_.bitcast(BF16)[:, 0:128], k_

    def mslot():
        a_, k_ = bank()
        return a_[:, 0:64], k_

    def pst(w):
        a, k = bank()
        return a[:, 0:w], k

    def mm(out, lhsT, rhs, start, stop, r, w):
        S.add("pe", lambda e: e.matmul(out, lhsT=lhsT, rhs=rhs, start=start, stop=stop), r=r, w=w)

    def tr(out, in_, idn, r, w):
        S.add("pe", lambda e: e.transpose(out, in_, idn), r=r, w=w)

    def act(out, in_, func, r, w, bias=None, scale=None):
        kw = {}
        if bias is not None:
            kw["bias"] = bias
        if scale is not None:
            kw["scale"] = scale
        S.add("act", lambda e: e.activation(out, in_, func, **kw), r=r, w=w)

    def tt(eng, out, in0, in1, op, r, w):
        S.add(eng, lambda e: e.tensor_tensor(out, in0, in1, op), r=r, w=w)

    def ts(eng, out, in0, s1, s2, op0, op1, r, w):
        if s2 is None:
            S.add(eng, lambda e: e.tensor_scalar(out, in0, s1, None, op0), r=r, w=w)
        else:
            S.add(eng, lambda e: e.tensor_scalar(out, in0, s1, s2, op0, op1), r=r, w=w)

    def stt(eng, out, in0, sc, in1, op0, op1, r, w):
        S.add(eng, lambda e: e.scalar_tensor_tensor(out, in0, sc, in1, op0, op1), r=r, w=w)

    def cp(eng, out, in_, r, w):
        if eng == "act":
            S.add("act", lambda e: e.copy(out, in_), r=r, w=w)
        else:
            S.add(eng, lambda e: e.tensor_copy(out, in_), r=r, w=w)

    def dma(q, out, in_, r, w):
        S.add(q, lambda e: e.dma_start(out=out, in_=in_), r=r, w=w, kind="d")

    def wload(src, n=WN):
        i = st["slot"] % NSLOT
        st["slot"] += 1
        dma("pool", WR[:, i, 0:n], src, r=[], w=[("W", i)])
        return i

    dma("sp", cst, cst_in, r=[], w=["cst"])
    dma("sp", lp, lp_in, r=[], w=["lp"])
    for kc in range(KC):
        dma("sp", xd[kc], xT[kc * 128:(kc + 1) * 128, :], r=[], w=[("xd", kc)])
    dma("sp", cTs, cT.rearrange("(k p) q -> p k q", p=128), r=[], w=["cTs"])
    dma("sp", identf, idn_in, r=[], w=["identf"])
    S.add("dve", lambda e: e.memset(ones, 1.0), w=["ones"])
    cp("dve", ident, identf, r=["identf"], w=["ident"])
    act(cTb, cTs, AF.Silu, r=["cTs"], w=["cTb"])
    abv = cst[:, C.c_ab:C.c_ab + AQ * 256].rearrange("p (h j) -> p h j", h=AQ)
    ts("dve", abias0, abv[:, :, 0:128], cst[:, C.c_negf:C.c_negf + 1], None, ALU.add, None,
       r=["cst"], w=["abias0"])
    decT = cst[:, C.c_decT:C.c_decT + RH * 128].rearrange("p (h i) -> p h i", h=RH)
    qdec = cst[:, C.c_qdec:C.c_qdec + RH * 128].rearrange("p (h i) -> p h i", h=RH)
    epsc = cst[:, C.c_eps:C.c_eps + 1]

    def lpv(l, off, n):
        return lp[:, l, off:off + n]

    def proj(slot, nk, actf, akeys, evac, wv=None, tiles=None):
        wv = wv if wv is not None else WR[:, slot, :].rearrange("p (k n) -> p k n", k=nk)
        for (c0, w) in (tiles or C.TT):
            ps, pk = pst(w)
            for k in range(nk):
                mm(ps, wv[:, k, :], actf(k, c0, w), k == 0, k == nk - 1, r=[("W", slot)] + akeys(k), w=pk)
            evac(ps, pk, c0, w)

    def hact(k, c0, w):
        return H[:, k, c0:c0 + w]

    def hkeys(k):
        return [("H", k)]

    def xload(k):
        i = st["xc"] % 2
        st["xc"] += 1
        dma("sp", XC[:, i, :], xd[k], r=[("xd", k)], w=[("XC", i)])
        return i

    def stat_begin():
        return [pst(w) for (c0, w) in C.TT]

    def stat_acc(pss, srcf, skeys, k, nk):
        i = st["sqi"] % 2
        st["sqi"] += 1
        for ti, (c0, w) in enumerate(C.TT):
            act(sq[:, i, c0:c0 + w], srcf(c0, w), AF.Square, r=skeys, w=[("sq", i, ti)])
            mm(pss[ti][0], ones, sq[:, i, c0:c0 + w], k == 0, k == nk - 1, r=[("sq", i, ti), "ones"], w=pss[ti][1])

    def stat_end(pss, D_):
        for ti, (c0, w) in enumerate(C.TT):
            act(Rn[:, c0:c0 + w], pss[ti][0], AF.Sqrt, r=pss[ti][1] + ["cst"], w=[("Rn", ti)], bias=epsc, scale=1.0 / D_)
            S.add("dve", (lambda a: (lambda e: e.reciprocal(a, a)))(Rn[:, c0:c0 + w]), r=[("Rn", ti)], w=[("Rn", ti)])

    def rn_keys():
        return [("Rn", ti) for ti in range(len(C.TT))]

    def stats_x():
        pss = stat_begin()
        for k in range(KC):
            i = xload(k)
            stat_acc(pss, lambda c0, w, i=i: XC[:, i, c0:c0 + w], [("XC", i)], k, KC)
        stat_end(pss, C.D)

    def stats_src(srcf, skeys):
        pss = stat_begin()
        for k in range(KC):
            stat_acc(pss, lambda c0, w, k=k: srcf(k, c0, w), skeys(k), k, KC)
        stat_end(pss, C.D)

    def osrc(k, c0, w):
        if c0 < C.SPL:
            return OA[:, k, c0:c0 + w]
        return OB[:, k, c0 - C.SPL:c0 - C.SPL + w]

    def fsrc(k, c0, w):
        return Fa[:, k, c0:c0 + w]

    def norm_apply(which, l):
        for k in range(KC):
            xi = xload(k)
            for (c0, w) in C.TT[:-1]:
                i = st["tmpa"] % 2
                st["tmpa"] += 1
                tt("dve", tmpA[:, i, 0:w], XC[:, xi, c0:c0 + w], Rn[:, c0:c0 + w], ALU.mult,
                   r=[("XC", xi)] + rn_keys(), w=[("tmpA", i)])
                act(H[:, k, c0:c0 + w], tmpA[:, i, 0:w], AF.Identity, r=[("tmpA", i), "mA", "modT"], w=[("H", k)],
                    bias=modT[:, (3 * which) * KC + k, 0:1], scale=mA[:, which, k, 0:1])
            tt("dve", tmpS[:, k, :], XC[:, xi, NP:NT], Rn[:, NP:NT], ALU.mult, r=[("XC", xi)] + rn_keys(), w=["tmpS"])
        tt("dve", tmpS, tmpS, mAs[:, which], ALU.mult, r=["tmpS", "mAs"], w=["tmpS"])
        tt("dve", H[:, :, NP:NT], tmpS, mBs[:, which], ALU.add, r=["tmpS", "mBs"], w=[("H", k) for k in range(KC)])

    def resid(which, srcf, skeys, dst):
        for k in range(KC):
            tt("dve", tmpS[:, k, :], srcf(k, NP, NS), Rn[:, NP:NT], ALU.mult, r=skeys(k) + rn_keys(), w=["tmpS"])
        tt("dve", tmpS, tmpS, mGs[:, which], ALU.mult, r=["tmpS", "mGs"], w=["tmpS"])
        xis = []
        for k in range(KC):
            xi = xload(k)
            xis.append(xi)
            for (c0, w) in C.TT[:-1]:
                i = st["tmpa"] % 2
                st["tmpa"] += 1
                tt("dve", tmpA[:, i, 0:w], srcf(k, c0, w), Rn[:, c0:c0 + w], ALU.mult,
                   r=skeys(k) + rn_keys(), w=[("tmpA", i)])
                stt("dve", XC[:, xi, c0:c0 + w], tmpA[:, i, 0:w], mG[:, which, k, 0:1], XC[:, xi, c0:c0 + w],
                    ALU.mult, ALU.add, r=[("tmpA", i), "mG", ("XC", xi)], w=[("XC", xi)])
            tt("dve", XC[:, xi, NP:NT], XC[:, xi, NP:NT], tmpS[:, k, :], ALU.add, r=["tmpS", ("XC", xi)], w=[("XC", xi)])
            dma("sp", dst(k), XC[:, xi, :], r=[("XC", xi)], w=[("xd", k)])

    def tok_major(h, n, c0, T, need_kd=True):
        ps, pk = tslot()
        tr(ps[0:T, :], vT[:, c0:c0 + T], ident, r=["vT", "ident"], w=pk)
        cp("act", vtok[0:T, n, :], ps[0:T, :], r=pk, w=[("vtok", n)])
        if need_kd:
            ps2, pk2 = tslot()
            tr(ps2[0:T, :], kT[:, c0:c0 + T], ident, r=["kT", "ident"], w=pk2)
            col = (C.c_kdP if T == 128 else C.c_kdS) + h
            ts("dve", kdtok[0:T, n, :], ps2[0:T, :], cst[0:T, col:col + 1], None, ALU.mult, None,
               r=pk2 + ["cst"], w=[("kdtok", n)])

    def state_update(h, n, T, cur, nxt, first, gT, s_in32=None, skey=None):
        ps, pk = bank()
        TP = 128
        mm(ps[:, 0:128], kdtok[0:TP, n, :], vtok[0:TP, n, :], True, True, r=[("kdtok", n), ("vtok", n)], w=pk)
        if first:
            cp("dve", S32[:, nxt, :], ps[:, 0:128], r=pk, w=[("S32", nxt)])
        else:
            src = S32[:, cur, :] if s_in32 is None else s_in32
            rk = [("S32", cur)] if s_in32 is None else skey
            stt("dve", S32[:, nxt, :], src, float(gT), ps[:, 0:128], ALU.mult, ALU.add, r=pk + rk, w=[("S32", nxt)])

    def ret_out(h, n, c0, T, sbf_ap, sbf_key):
        ps, pk = bank()
        mm(ps[0:T, 0:T], kT[:, c0:c0 + T], qT[:, c0:c0 + T], True, True, r=["kT", "qT"], w=pk)
        i = n % 2
        tt("dve", sTt[0:T, i, 0:T], ps[0:T, 0:T], decT[0:T, h, 0:T], ALU.mult, r=pk + ["cst"], w=[("sT", i)])
        KV = int(os.environ.get("K_V", "9"))
        if T < 32 and KV < 2:
            return
        po, pok = bank()
        TP = 128
        mm(po[:, 0:T], vtok[0:TP, n, :], sTt[0:TP, i, 0:T], True, sbf_ap is None or (T < 32 and KV < 3), r=[("vtok", n), ("sT", i)], w=pok)
        if T < 32 and KV < 3:
            cp("act", ry[:, c0:c0 + T], po[:, 0:T], r=pok, w=["ry"])
            return
        if sbf_ap is not None:
            mm(po[:, 0:T], sbf_ap, qdT[:, c0:c0 + T], False, True, r=sbf_key + ["qdT"], w=pok)
        cp("act", ry[:, c0:c0 + T], po[:, 0:T], r=pok, w=["ry"])

    def ada(l):
        for q in range(6 * KC):
            slot = wload(w_ada[l, q])
            ps, pk = mslot()
            wv = WR[:, slot, :].rearrange("p (k n) -> p k n", k=KC)
            for k in range(KC):
                mm(ps[:, 0:NQ], wv[:, k, :], cTb[:, k, :], k == 0, k == KC - 1, r=[("W", slot), "cTb"], w=pk)
            act(modT[:, q, :], ps[:, 0:NQ], AF.Identity, r=pk + ["lp"], w=["modT"],
                bias=lp[:, l, C.l_ba + q:C.l_ba + q + 1], scale=1.0)
        ng = lpv(l, C.l_ng, 4 * KC).rearrange("p (a k) -> p a k", a=4)
        for which in range(2):
            scv = modT[:, (3 * which + 1) * KC:(3 * which + 2) * KC, :]
            gv = modT[:, (3 * which + 2) * KC:(3 * which + 3) * KC, :]
            shv = modT[:, (3 * which) * KC:(3 * which + 1) * KC, :]
            ts("dve", mA[:, which], scv, 1.0, None, ALU.add, None, r=["modT"], w=["mA"])
            tt("dve", mA[:, which], mA[:, which], ng[:, 2 * which, :].unsqueeze(2).broadcast_to([128, KC, NQ]), ALU.mult,
               r=["mA", "lp"], w=["mA"])
            tt("dve", mG[:, which], gv, ng[:, 2 * which + 1, :].unsqueeze(2).broadcast_to([128, KC, NQ]), ALU.mult,
               r=["modT", "lp"], w=["mG"])
            for (dst, src, key) in ((mAs, mA[:, which], "mAs"), (mBs, shv, "mBs"), (mGs, mG[:, which], "mGs")):
                cp("dve", dst[:, which].rearrange("p k (b t) -> p k b t", t=TS),
                   src[:, :, 1:NQ].unsqueeze(3).broadcast_to([128, KC, NSQ, TS]),
                   r=["mA", "mG", "modT"], w=[key])

    def layers():
      for l in range(DEPTH):
          chk(0)
          ada(l)
          chk(1)
          for b in range(NSQ):
              for hk in range(2):
                  dma("sp", o_wks[l, b, hk, :, 0:128 - TS], c_kd[l, b, hk, 0:64, TS:128], r=[], w=[])
              dma("sp", o_wvs[l, b, :, 0:128 - TS], c_vT[l, b, :, TS:128], r=[], w=[])
          if l == 0:
              stats_x()
          norm_apply(0, l)
          bi = 0

          def nextblk():
              nonlocal bi
              s_ = wload(w_in[l, bi])
              bi += 1
              return s_

          cw = lpv(l, C.l_cw, 3 * CCH).rearrange("p (j c) -> p j c", j=3)
          chk(2)
          for h in range(RH):
              sk = nextblk()
              proj(sk, KC, hact, hkeys, lambda ps, pk, c0, w: act(kT[:, c0:c0 + w], ps, AF.Identity, r=pk, w=["kT"], scale=128.0 ** -0.5),
                   tiles=C.TT[:-1])
              sv = nextblk()
              proj(sv, KC, hact, hkeys, lambda ps, pk, c0, w: cp("act", vT[:, c0:c0 + w], ps, r=pk, w=["vT"]), tiles=C.TT[:-1])
              for n in range(NCH):
                  tok_major(h, n, n * 128, 128)
                  state_update(h, n, 128, n % 2, (n + 1) % 2, n == 0, C.gam[h] ** 128)
              cp("act", STG[:, C.b_S + h * 128:C.b_S + (h + 1) * 128], S32[:, NCH % 2, :], r=[("S32", NCH % 2)], w=["STG"])
          chk(3)
          for hk in range(2):
              sk = nextblk()

              def ev_k(ps, pk, c0, w, hk=hk):
                  cp("act", kk[:, hk, c0:c0 + w], ps, r=pk, w=[("kk", hk)])
                  if c0 + w == NP:
                      cp("act", k32[:, hk, 0:128], ps[:, w - 128:w], r=pk, w=[("k32", hk)])
                  if c0 == NP:
                      cp("act", k32[:, hk, 128:128 + NS], ps, r=pk, w=[("k32", hk)])
              proj(sk, KC, hact, hkeys, ev_k)
              cp("dve", STG[:, C.b_k + hk * 128:C.b_k + (hk + 1) * 128], kk[:, hk, NP - 128:NP], r=[("kk", hk)], w=["STG"])
              dma("sp", o_wkp[l, hk], k32[0:64, hk, 0:128], r=[("k32", hk)], w=[])
              for b in range(NSQ):
                  if os.environ.get("K_NOSMALL"):
                      continue
                  dma("sp", o_wks[l, b, hk, :, 128 - TS:128], k32[0:64, hk, 128 + b * TS:128 + (b + 1) * TS], r=[("k32", hk)], w=[])
          chk(31)
          sv = nextblk()

          def ev_v(ps, pk, c0, w):
              cp("act", vTa[:, c0:c0 + w], ps, r=pk, w=["vTa"])
              if c0 + w == NP:
                  cp("act", v32[:, 0:128], ps[:, w - 128:w], r=pk, w=["v32"])
              if c0 == NP:
                  cp("act", v32[:, 128:128 + NS], ps, r=pk, w=["v32"])
          proj(sv, KC, hact, hkeys, ev_v)
          dma("sp", o_wvp[l], v32[:, 0:128], r=["v32"], w=[])
          for b in range(NSQ):
              dma("sp", o_wvs[l, b, :, 128 - TS:128], v32[:, 128 + b * TS:128 + (b + 1) * TS], r=["v32"], w=[])
          chk(32)
          S.add("dve", lambda e: e.memset(vtoka[:, NCH:NCH + NSQ, :], 0.0), w=[("vtoka", NCH + b_) for b_ in range(NSQ)])
          for n in range(NCH + NSQ):
              c0, T = (n * 128, 128) if n < NCH else (NP + (n - NCH) * TS, TS)
              ps, pk = tslot()
              tr(ps[0:T, :], vTa[:, c0:c0 + T], ident, r=["vTa", "ident"], w=pk)
              cp("act", vtoka[0:T, n, :], ps[0:T, :], r=pk, w=[("vtoka", n)])
          cp("dve", STG[:, C.b_v:C.b_v + 128], vtoka[:, NCH - 1, :], r=[("vtoka", NCH - 1)], w=["STG"])
          chk(33)
          for c in range(CCH):
              dma("sp", Us[:, :, 0:2], st_cv[l, :, c], r=[], w=["Us"])
              sb_ = nextblk()
              proj(sb_, KC, hact, hkeys, lambda ps, pk, c0, w: cp("act", cb32[:, c0:c0 + w], ps, r=pk, w=["cb32"]))
              sc_ = nextblk()
              proj(sc_, KC, hact, hkeys, lambda ps, pk, c0, w: cp("act", cc32[:, c0:c0 + w], ps, r=pk, w=["cc32"]))
              sh_ = nextblk()
              S.add("dve", lambda e: e.memset(Up[:, 0:2], 0.0), w=["Up"])

              def ev_h(ps, pk, c0, w):
                  if c0 < NP:
                      tt("dve", Up[:, 2 + c0:2 + c0 + w], cc32[:, c0:c0 + w], ps, ALU.mult, r=pk + ["cc32"], w=["Up"])
                  else:
                      tt("dve", Us[:, :, 2:2 + TS], cc32[:, NP:NT].rearrange("p (b t) -> p b t", t=TS),
                         ps.rearrange("p (b t) -> p b t", t=TS), ALU.mult, r=pk + ["cc32"], w=["Us"])
              proj(sh_, KC, hact, hkeys, ev_h)
              mc = RH + AQ // 2 + c
              ts("dve", cvt1[:, 0:NP], Up[:, 0:NP], cw[:, 0, c:c + 1], None, ALU.mult, None, r=["Up", "lp"], w=["cvt1"])
              stt("dve", cvt1[:, 0:NP], Up[:, 1:NP + 1], cw[:, 1, c:c + 1], cvt1[:, 0:NP], ALU.mult, ALU.add, r=["Up", "lp", "cvt1"], w=["cvt1"])
              stt("dve", cvt1[:, 0:NP], Up[:, 2:NP + 2], cw[:, 2, c:c + 1], cvt1[:, 0:NP], ALU.mult, ALU.add, r=["Up", "lp", "cvt1"], w=["cvt1"])
              tt("dve", M[:, mc, 0:NP], cvt1[:, 0:NP], cb32[:, 0:NP], ALU.mult, r=["cvt1", "cb32"], w=[("M", mc)])
              c3 = cvt1[:, NP:NT].rearrange("p (b t) -> p b t", t=TS)
              ts("dve", c3, Us[:, :, 0:TS], cw[:, 0, c:c + 1], None, ALU.mult, None, r=["Us", "lp"], w=["cvt1"])
              stt("dve", c3, Us[:, :, 1:TS + 1], cw[:, 1, c:c + 1], c3, ALU.mult, ALU.add, r=["Us", "lp", "cvt1"], w=["cvt1"])
              stt("dve", c3, Us[:, :, 2:TS + 2], cw[:, 2, c:c + 1], c3, ALU.mult, ALU.add, r=["Us", "lp", "cvt1"], w=["cvt1"])
              tt("dve", M[:, mc, NP:NT], cvt1[:, NP:NT], cb32[:, NP:NT], ALU.mult, r=["cvt1", "cb32"], w=[("M", mc)])
              cp("dve", cbF[:, c, :], cb32[:, 0:2], r=["cb32"], w=["cbF"])
              cp("dve", uF[:, c, :], Up[:, 2:4], r=["Up"], w=["uF"])
              cp("dve", STG[:, C.b_u + 2 * c:C.b_u + 2 * c + 2], Up[:, NP:NP + 2], r=["Up"], w=["STG"])
              dma("sp", o_cvp[l, :, c, :], Up[:, NP:NP + 2], r=["Up"], w=[])
              dma("sp", o_cvs[l, :, c], Us[:, :, TS:TS + 2], r=["Us"], w=[])
          chk(4)
          dma("sp", bounce[l][:, :], STG, r=["STG"], w=["bounce"])
          S.add("pool", (lambda l: (lambda e: e.collective_compute(
              "AllGather", ALU.bypass, replica_groups=[list(range(NCORE))],
              ins=[bounce[l].ap().opt()], outs=[gath[l].ap().opt()])))(l), r=["bounce"], w=["gath"], kind="cc")
          gv = gath[l].ap().rearrange("(r p) c -> p r c", p=128)
          coef = cst[:, C.c_coef:C.c_coef + NCORE * RH].rearrange("p (r h) -> p r h", r=NCORE)
          oh = cst[:, C.c_oh:C.c_oh + NCORE]

          def combine(dst32, cols, scal, dkey, width=128):
              dma("sp", Gt[:, :, 0:width], gv[:, :, cols:cols + width], r=["gath"], w=["Gt"])
              for r_ in range(NCORE):
                  if r_ == 0:
                      ts("dve", dst32, Gt[:, 0, 0:width], scal(0), None, ALU.mult, None, r=["Gt", "cst"], w=[dkey])
                  else:
                      stt("dve", dst32, Gt[:, r_, 0:width], scal(r_), dst32, ALU.mult, ALU.add, r=["Gt", "cst", dkey], w=[dkey])

          for h in range(RH):
              combine(Sst[:, h, :], C.b_S + h * 128, lambda r_, h=h: coef[:, r_, h:h + 1], ("Sst", h))
          for hk in range(2):
              combine(ptmp, C.b_k + hk * 128, lambda r_: oh[:, r_:r_ + 1], "ptmp")
              cp("dve", pK[:, hk, :], ptmp, r=["ptmp"], w=["pK"])
          combine(ptmp, C.b_v, lambda r_: oh[:, r_:r_ + 1], "ptmp")
          cp("dve", pV, ptmp, r=["ptmp"], w=["pV"])
          dma("sp", Gu, gv[:, :, C.b_u:C.b_u + 2 * CCH], r=["gath"], w=["Gu"])
          utf = utail.rearrange("p c t -> p (c t)")
          for r_ in range(NCORE):
              if r_ == 0:
                  ts("dve", utf, Gu[:, 0, :], oh[:, 0:1], None, ALU.mult, None, r=["Gu", "cst"], w=["utail"])
              else:
                  stt("dve", utf, Gu[:, r_, :], oh[:, r_:r_ + 1], utf, ALU.mult, ALU.add, r=["Gu", "cst", "utail"], w=["utail"])
          cwT = cw.rearrange("p j c -> p c j")
          tt("dve", fix[:, :, 0], utail[:, :, 0], cwT[:, :, 0], ALU.mult, r=["utail", "lp"], w=["fix"])
          tt("dve", fix[:, :, 2], utail[:, :, 1], cwT[:, :, 1], ALU.mult, r=["utail", "lp"], w=["fix"])
          tt("dve", fix[:, :, 0], fix[:, :, 0], fix[:, :, 2], ALU.add, r=["fix"], w=["fix"])
          tt("dve", fix[:, :, 2], uF[:, :, 0], cwT[:, :, 2], ALU.mult, r=["uF", "lp"], w=["fix"])
          tt("dve", fix[:, :, 0], fix[:, :, 0], fix[:, :, 2], ALU.add, r=["fix"], w=["fix"])
          tt("dve", fix[:, :, 1], utail[:, :, 1], cwT[:, :, 0], ALU.mult, r=["utail", "lp"], w=["fix"])
          tt("dve", fix[:, :, 2], uF[:, :, 0], cwT[:, :, 1], ALU.mult, r=["uF", "lp"], w=["fix"])
          tt("dve", fix[:, :, 1], fix[:, :, 1], fix[:, :, 2], ALU.add, r=["fix"], w=["fix"])
          tt("dve", fix[:, :, 2], uF[:, :, 1], cwT[:, :, 2], ALU.mult, r=["uF", "lp"], w=["fix"])
          tt("dve", fix[:, :, 1], fix[:, :, 1], fix[:, :, 2], ALU.add, r=["fix"], w=["fix"])
          for c in range(CCH):
              mc = RH + AQ // 2 + c
              tt("dve", M[:, mc, 0:2], fix[:, c, 0:2], cbF[:, c, :], ALU.mult, r=["fix", "cbF"], w=[("M", mc)])
          chk(5)
          for h in range(RH):
              s_ = nextblk()

              def ev_q(ps, pk, c0, w, h=h):
                  cp("act", qT[:, c0:c0 + w], ps, r=pk, w=["qT"])
                  if c0 < NP:
                      tt("dve", qdT[:, c0:c0 + w].rearrange("p (n i) -> p n i", i=128), ps.rearrange("p (n i) -> p n i", i=128),
                         qdec[:, h, :].unsqueeze(1).broadcast_to([128, w // 128, 128]), ALU.mult, r=pk + ["cst"], w=["qdT"])
                  else:
                      tt("dve", qdT[:, c0:c0 + w].rearrange("p (b t) -> p b t", t=TS), ps.rearrange("p (b t) -> p b t", t=TS),
                         qdec[:, h, 0:TS].unsqueeze(1).broadcast_to([128, NSQ, TS]), ALU.mult, r=pk + ["cst"], w=["qdT"])
              proj(s_, KC, hact, hkeys, ev_q)
              s_ = nextblk()
              proj(s_, KC, hact, hkeys, lambda ps, pk, c0, w: act(kT[:, c0:c0 + w], ps, AF.Identity, r=pk, w=["kT"], scale=128.0 ** -0.5))
              s_ = nextblk()
              proj(s_, KC, hact, hkeys, lambda ps, pk, c0, w: cp("act", vT[:, c0:c0 + w], ps, r=pk, w=["vT"]))
              s_ = nextblk()
              proj(s_, KC, hact, hkeys, lambda ps, pk, c0, w: act(gsl[:, c0:c0 + w], ps, AF.Silu, r=pk, w=["gsl"]))
              cp("dve", S32[:, 0, :], Sst[:, h, :], r=[("Sst", h)], w=[("S32", 0)])
              for n in range(NCH):
                  cur, nxt = n % 2, (n + 1) % 2
                  cp("act", Sbf[:, cur, :], S32[:, cur, :], r=[("S32", cur)], w=[("Sbf", cur)])
                  tok_major(h, n, n * 128, 128)
                  ret_out(h, n, n * 128, 128, Sbf[:, cur, :], [("Sbf", cur)])
                  state_update(h, n, 128, cur, nxt, False, C.gam[h] ** 128)
              dma("sp", o_retp[l, h], S32[:, NCH % 2, :], r=[("S32", NCH % 2)], w=[])
              skeys_ = [("vtok", NCH + b_) for b_ in range(NSQ)] + [("kdtok", NCH + b_) for b_ in range(NSQ)] + [("sT", 0), ("sT", 1)]
              S.add("dve", lambda e: e.memset(vtok[:, NCH:NCH + NSQ, :], 0.0), w=skeys_[0:NSQ])
              S.add("dve", lambda e: e.memset(kdtok[:, NCH:NCH + NSQ, :], 0.0), w=skeys_[NSQ:2 * NSQ])
              S.add("dve", lambda e: e.memset(sTt[:, :, :], 0.0), w=[("sT", 0), ("sT", 1)])
              for b in range(NSQ):
                  n = NCH + b
                  i = b % 2
                  c0 = NP + b * TS
                  SL = int(os.environ.get("K_SL", "9"))
                  if SL < 1:
                      continue
                  dma("sp", s0f[:, i, :], st_ret[l, b, h], r=[], w=[("s0f", i)])
                  cp("act", Sbf[:, i, :], s0f[:, i, :], r=[("s0f", i)], w=[("Sbf", i)])
                  if SL < 2:
                      continue
                  tok_major(h, n, c0, TS)
                  if SL < 3:
                      continue
                  ret_out(h, n, c0, TS, Sbf[:, i, :], [("Sbf", i)])
                  if SL < 4:
                      continue
                  state_update(h, n, TS, i, i, False, C.gam[h] ** TS, s_in32=s0f[:, i, :], skey=[("s0f", i)])
                  dma("sp", o_rets[l, b, h], S32[:, i, :], r=[("S32", i)], w=[])
              pss = [pst(w) for (c0, w) in C.TT]
              for ti, (c0, w) in enumerate(C.TT):
                  act(sq[:, 0, c0:c0 + w], ry[:, c0:c0 + w], AF.Square, r=["ry"], w=[("sq", 0, ti)])
                  mm(pss[ti][0], ones, sq[:, 0, c0:c0 + w], True, True, r=[("sq", 0, ti), "ones"], w=pss[ti][1])
                  act(cvt1[:, c0:c0 + w], pss[ti][0], AF.Sqrt, r=pss[ti][1] + ["cst"], w=["cvt1"], bias=epsc, scale=1.0 / 128)
                  S.add("dve", (lambda a: (lambda e: e.reciprocal(a, a)))(cvt1[:, c0:c0 + w]), r=["cvt1"], w=["cvt1"])
                  tt("dve", ry[:, c0:c0 + w], ry[:, c0:c0 + w], cvt1[:, c0:c0 + w], ALU.mult, r=["ry", "cvt1"], w=["ry"])
                  tt("dve", M[:, h, c0:c0 + w], ry[:, c0:c0 + w], gsl[:, c0:c0 + w], ALU.mult, r=["ry", "gsl"], w=[("M", h)])
          chk(6)
          for c in range(AQ // 2):
              s_ = nextblk()
              proj(s_, KC, hact, hkeys, lambda ps, pk, c0, w, c=c: act(qTa[:, c, c0:c0 + w], ps, AF.Identity, r=pk, w=[("qTa", c)], scale=0.125))
          sinks = lpv(l, C.l_sk, AQ)
          for n in range(NCH + NSQ):
              prompt = n < NCH
              c0, T = (n * 128, 128) if prompt else (NP + (n - NCH) * TS, TS)
              W_ = 128 + T
              if not prompt:
                  b = n - NCH
                  i = b % 2
                  for hk in range(2):
                      dma("pool", ckk[:, i, hk, :], c_kd[l, b, hk], r=[], w=[("ckk", i)])
                  dma("pool", cvt[:, i, :], c_v[l, b], r=[], w=[("cvt", i)])
              for hk in range(2):
                  sp_, spk = pair()
                  s4 = sp_.rearrange("p (lo hi j) -> p hi lo j", lo=2, hi=2)

                  def hl(a):
                      return a.rearrange("p (hi lo) j -> p hi lo j", lo=2)
                  for g in range(GRP):
                      c = (hk * GRP + g) // 2
                      e_ = (hk * GRP + g) % 2
                      lq = qTa[64 * e_:64 * e_ + 64, c, c0:c0 + T]
                      if prompt and n > 0:
                          kp, kpk = kk[64 * e_:64 * e_ + 64, hk, c0 - 128:c0], [("kk", hk)]
                      elif prompt:
                          kp, kpk = pK[64 * e_:64 * e_ + 64, hk, :], ["pK"]
                      else:
                          kp, kpk = ckk[64 * e_:64 * e_ + 64, i, hk, :], [("ckk", i)]
                      mm(s4[0:T, g // 2, g % 2, 0:128], lq, kp, True, True, r=[("qTa", c)] + kpk, w=spk)
                      mm(s4[0:T, g // 2, g % 2, 128:W_], lq, kk[64 * e_:64 * e_ + 64, hk, c0:c0 + T], True, True, r=[("qTa", c), ("kk", hk)], w=spk)
                  bprev = abias0[0:T, hk * GRP:(hk + 1) * GRP, :] if (prompt and n == 0) else abv[0:T, hk * GRP:(hk + 1) * GRP, 0:128]
                  tt("dve", hl(sbt[0:T, :, 0:128]), s4[0:T, 0:GRP // 2, :, 0:128], hl(bprev), ALU.add, r=spk + ["abias0", "cst"], w=["sbt"])
                  tt("dve", hl(sbt[0:T, :, 128:W_]), s4[0:T, 0:GRP // 2, :, 128:W_], hl(abv[0:T, hk * GRP:(hk + 1) * GRP, 128:W_]), ALU.add,
                     r=spk + ["cst"], w=["sbt"])
                  S.add("dve", (lambda T=T, W_=W_: (lambda e: e.tensor_reduce(mx[0:T, 0, :], sbt[0:T, :, 0:W_], AX.X, ALU.max)))(),
                        r=["sbt"], w=["mx"])
                  tt("dve", mx[0:T, 0, :], mx[0:T, 0, :], sinks[0:T, hk * GRP:(hk + 1) * GRP], ALU.max, r=["mx", "lp"], w=["mx"])
                  tt("dve", sbt[0:T, :, 0:W_], sbt[0:T, :, 0:W_], mx[0:T, 0, :].unsqueeze(2).broadcast_to([T, GRP, W_]), ALU.subtract,
                     r=["sbt", "mx"], w=["sbt"])
                  act(pbt[0:T, :, 0:W_], sbt[0:T, :, 0:W_], AF.Exp, r=["sbt"], w=["pbt"])
                  S.add("dve", (lambda T=T, W_=W_: (lambda e: e.tensor_reduce(mx[0:T, 1, :], pbt[0:T, :, 0:W_], AX.X, ALU.add)))(),
                        r=["pbt"], w=["mx"])
                  tt("dve", mx[0:T, 2, :], sinks[0:T, hk * GRP:(hk + 1) * GRP], mx[0:T, 0, :], ALU.subtract, r=["mx", "lp"], w=["mx"])
                  act(mx[0:T, 2, :], mx[0:T, 2, :], AF.Exp, r=["mx"], w=["mx"])
                  tt("dve", mx[0:T, 1, :], mx[0:T, 1, :], mx[0:T, 2, :], ALU.add, r=["mx"], w=["mx"])
                  S.add("dve", (lambda T=T: (lambda e: e.reciprocal(mx[0:T, 3, :], mx[0:T, 1, :])))(), r=["mx"], w=["mx"])
                  if T < 32:
                      S.add("dve", lambda e: e.memset(pTt[:, :, 1, 0:32], 0.0), r=[], w=["pTt"])
                  for g in range(GRP):
                      ps, pk = tslot()
                      TP = max(T, 32)
                      tr(ps[:, 0:TP], pbt[0:TP, g, 0:128], ident[0:TP, 0:TP], r=["pbt", "ident"], w=pk)
                      cp("act", pTt[:, g, 0, 0:T], ps[:, 0:T], r=pk, w=["pTt"])
                      ps2, pk2 = tslot()
                      tr(ps2[0:T, 0:TP], pbt[0:TP, g, 128:W_], ident[0:TP, 0:TP], r=["pbt", "ident"], w=pk2)
                      cp("act", pTt[0:T, g, 1, 0:T], ps2[0:T, 0:T], r=pk2, w=["pTt"])
                  po, pok = bank()
                  TP = 128
                  for g in range(GRP):
                      if prompt and n > 0:
                          vp, vpk = vtoka[:, n - 1, hk * 64:(hk + 1) * 64], [("vtoka", n - 1)]
                      elif prompt:
                          vp, vpk = pV[:, hk * 64:(hk + 1) * 64], ["pV"]
                      else:
                          vp, vpk = cvt[:, i, hk * 64:(hk + 1) * 64], [("cvt", i)]
                      mm(po[0:T, g * 64:(g + 1) * 64], pTt[:, g, 0, 0:T], vp, True, False, r=["pTt"] + vpk, w=pok)
                      mm(po[0:T, g * 64:(g + 1) * 64], pTt[0:TP, g, 1, 0:T], vtoka[0:TP, n, hk * 64:(hk + 1) * 64], False, True,
                         r=["pTt", ("vtoka", n)], w=pok)
                  tt("dve", ont[0:T], po[0:T, 0:GRP * 64].rearrange("p (g d) -> p g d", g=GRP),
                     mx[0:T, 3, :].unsqueeze(2).broadcast_to([T, GRP, 64]), ALU.mult, r=pok + ["mx"], w=["ont"])
                  for cc_ in range(GRP // 2):
                      ps, pk = tslot()
                      tr(ps[:, 0:TP], ont[0:TP, 2 * cc_:2 * cc_ + 2, :].rearrange("p g d -> p (g d)"), ident[0:TP, 0:TP], r=["ont", "ident"], w=pk)
                      mc = RH + hk * (GRP // 2) + cc_
                      cp("act", M[:, mc, c0:c0 + T], ps[:, 0:T], r=pk, w=[("M", mc)])
          assert bi == C.NB_IN
          chk(7)
          def mact(k, c0, w):
              return M[:, k, c0:c0 + w]

          for oc in range(KC):
              s_ = wload(w_out[l, oc])

              def ev_o(ps, pk, c0, w, oc=oc):
                  if c0 < C.SPL:
                      cp("act", OA[:, oc, c0:c0 + w], ps, r=pk, w=[("OA", oc)])
                  else:
                      cp("act", OB[:, oc, c0 - C.SPL:c0 - C.SPL + w], ps, r=pk, w=[("OB", oc)])
              proj(s_, KC, mact, lambda k: [("M", k)], ev_o)
          okeys = lambda k: [("OA", k), ("OB", k)]
          stats_src(osrc, okeys)
          resid(0, osrc, okeys, lambda k: xd[k])
          stats_x()
          chk(8)
          norm_apply(1, l)
          hc0 = 0
          for gi, gsz in enumerate(C.GROUPS):
              for j in range(gsz):
                  hc = hc0 + j
                  sg_ = wload(w_gate[l, hc])
                  su_ = wload(w_up[l, hc])
                  wg = WR[:, sg_, :].rearrange("p (k n) -> p k n", k=KC)
                  wu = WR[:, su_, :].rearrange("p (k n) -> p k n", k=KC)
                  for (c0, w) in C.TT:
                      pg, pgk = pst(w)
                      for k in range(KC):
                          mm(pg, wg[:, k, :], H[:, k, c0:c0 + w], k == 0, k == KC - 1, r=[("W", sg_), ("H", k)], w=pgk)
                      pu, puk = pst(w)
                      for k in range(KC):
                          mm(pu, wu[:, k, :], H[:, k, c0:c0 + w], k == 0, k == KC - 1, r=[("W", su_), ("H", k)], w=puk)
                      i = st["tmpa"] % 2
                      st["tmpa"] += 1
                      act(tmpA[:, i, 0:w], pg, AF.Silu, r=pgk, w=[("tmpA", i)])
                      tt("dve", HID[:, j, c0:c0 + w], tmpA[:, i, 0:w], pu, ALU.mult, r=[("tmpA", i)] + puk, w=[("HID", j)])
              for oc in range(KC):
                  sd_ = wload(w_down[l, gi, oc, :, 0:gsz * 128], n=gsz * 128)
                  wd = WR[:, sd_, 0:gsz * 128].rearrange("p (k n) -> p k n", k=gsz)
                  for (c0, w) in C.TT:
                      ps, pk = pst(w)
                      for j in range(gsz):
                          mm(ps, wd[:, j, :], HID[:, j, c0:c0 + w], j == 0, j == gsz - 1, r=[("W", sd_), ("HID", j)], w=pk)
                      if gi == 0:
                          cp("act", Fa[:, oc, c0:c0 + w], ps, r=pk, w=[("F", oc)])
                      else:
                          tt("dve", Fa[:, oc, c0:c0 + w], Fa[:, oc, c0:c0 + w], ps, ALU.add, r=pk + [("F", oc)], w=[("F", oc)])
              hc0 += gsz
          fkeys = lambda k: [("F", k)]
          stats_src(fsrc, fkeys)
          if l == DEPTH - 1:
              resid(1, fsrc, fkeys, lambda k: yT[k * 128:(k + 1) * 128, :])
          else:
              resid(1, fsrc, fkeys, lambda k: xd[k])
              stats_x()


    try:
        layers()
    except StopBuild:
        pass
    NDS = 12
    esem = {e: nc.alloc_semaphore(f"es_{e}") for e in Sched.ENG}
    dsems = {q: [nc.alloc_semaphore(f"ds_{q}{i}") for i in range(NDS)] for q in ("pool", "sp")}
    ccsem = nc.alloc_semaphore("ccsem")
    with nc.Block() as block:
        S.emit(nc, block, esem, dsems, ccsem)
    return nc


def make_consts(C, core):
    cs = np.zeros((128, C.NCST), np.float64)
    seq, seg = core // C.SEGS, core % C.SEGS
    i = np.arange(128)
    for h in range(C.RH):
        g = C.gam[h]
        lg = np.log1p(-2.0 ** (-5 - h))
        d = i[None, :] - i[:, None]
        cs[:, C.c_decT + h * 128:C.c_decT + (h + 1) * 128] = np.where(d >= 0, np.exp(np.maximum(d, 0) * lg), 0.0)
        cs[:, C.c_qdec + h * 128:C.c_qdec + (h + 1) * 128] = np.exp((i + 1) * lg)[None, :]
        cs[:, C.c_kdP + h] = np.exp((127 - i) * lg)
        cs[:C.TS, C.c_kdS + h] = np.exp((C.TS - 1 - i[:C.TS]) * lg)
        for r in range(NCORE):
            rs, rg = r // C.SEGS, r % C.SEGS
            if rs == seq and rg < seg:
                cs[:, C.c_coef + r * C.RH + h] = np.exp(C.NP * (seg - 1 - rg) * lg)
    for h in range(C.AQ):
        sl = C.slopes[h]
        ab = np.zeros((128, 256))
        qi = i[:, None]
        kj = i[None, :]
        dist = 128 + qi - kj
        ab[:, 0:128] = np.where(kj > qi, -sl * dist, NEG)
        dist = qi - kj
        ab[:, 128:256] = np.where(kj <= qi, -sl * dist, NEG)
        cs[:, C.c_ab + h * 256:C.c_ab + (h + 1) * 256] = ab
    if seg > 0:
        cs[:, C.c_oh + core - 1] = 1.0
    cs[:, C.c_negf] = 0.0 if seg > 0 else NEG
    cs[:, C.c_eps] = EPS
    return cs.astype(np.float32)


def blockify(w, cols):
    K = w.shape[0]
    t = w[:, cols].reshape(K // 128, 128, len(cols)).transpose(1, 0, 2)
    return np.ascontiguousarray(t).reshape(128, -1)


def prep_shared(C, inp):
    D, KC = C.D, C.KC
    sh = {}
    w_in = inp["w_in"]
    sh["w_in"] = np.stack([np.stack([blockify(w_in[l], C.wcol(k, i)) for (k, i) in C.BL]) for l in range(C.DEPTH)])
    sh["w_out"] = np.stack([np.stack([blockify(inp["w_out"][l], np.arange(128) + 128 * oc) for oc in range(KC)]) for l in range(C.DEPTH)])
    sh["w_ada"] = np.stack([np.stack([blockify(inp["w_ada"][l], np.arange(128) + 128 * q) for q in range(6 * KC)]) for l in range(C.DEPTH)])
    sh["w_gate"] = np.stack([np.stack([blockify(inp["w_ff_gate"][l], np.arange(128) + 128 * q) for q in range(C.FC)]) for l in range(C.DEPTH)])
    sh["w_up"] = np.stack([np.stack([blockify(inp["w_ff_up"][l], np.arange(128) + 128 * q) for q in range(C.FC)]) for l in range(C.DEPTH)])
    wd = np.zeros((C.DEPTH, len(C.GROUPS), KC, 128, C.GMAX * 128), np.float32)
    for l in range(C.DEPTH):
        r0 = 0
        for gi, gsz in enumerate(C.GROUPS):
            rows = inp["w_ff_down"][l][r0 * 128:(r0 + gsz) * 128]
            for oc in range(KC):
                wd[l, gi, oc, :, 0:gsz * 128] = blockify(rows, np.arange(128) + 128 * oc)
            r0 += gsz
    sh["w_down"] = wd
    lp = np.zeros((128, C.DEPTH, C.NLP), np.float32)
    for l in range(C.DEPTH):
        lp[:, l, C.l_ng:C.l_ng + 4 * KC] = inp["norm_g"][l].reshape(4, KC, 128).transpose(2, 0, 1).reshape(128, -1)
        lp[:, l, C.l_ba:C.l_ba + 6 * KC] = inp["b_ada"][l].reshape(6 * KC, 128).T
        lp[:, l, C.l_cw:C.l_cw + 3 * C.CCH] = inp["conv_w"][l].reshape(3, C.CCH, 128).transpose(2, 0, 1).reshape(128, -1)
        lp[:, l, C.l_sk:C.l_sk + C.AQ] = inp["attn_sinks"][l][None, :]
    sh["lp"] = lp
    return sh


def prep_core(C, inp, core, sh):
    seq, seg = core // C.SEGS, core % C.SEGS
    b0 = core * C.NSQ
    m = dict(sh)
    xp = inp["x_prompt"][seq, seg * C.NP:(seg + 1) * C.NP]
    xs = inp["x_sample"][b0:b0 + C.NSQ].reshape(C.NS, C.D)
    m["xT"] = np.ascontiguousarray(np.concatenate([xp, xs], 0).T)
    cc = np.concatenate([inp["c_prompt"][seq:seq + 1], inp["c_sample"][b0:b0 + C.NSQ]], 0)
    m["cT"] = np.ascontiguousarray(cc.T)
    m["cst"] = make_consts(C, core)
    m["idn"] = np.eye(128, dtype=np.float32)
    m["st_ret"] = np.ascontiguousarray(inp["state_ret"][:, b0:b0 + C.NSQ])
    ck = inp["cache_win_k"][:, b0:b0 + C.NSQ]
    ckT = ck.transpose(0, 1, 3, 4, 2)
    m["c_kd"] = np.ascontiguousarray(np.concatenate([ckT, ckT], axis=3))
    cv = inp["cache_win_v"][:, b0:b0 + C.NSQ].reshape(C.DEPTH, C.NSQ, 128, 128)
    m["c_v"] = np.ascontiguousarray(cv)
    m["c_vT"] = np.ascontiguousarray(cv.transpose(0, 1, 3, 2))
    sc = inp["state_conv"][:, b0:b0 + C.NSQ]
    m["st_cv"] = np.ascontiguousarray(sc.reshape(C.DEPTH, C.NSQ, 2, C.CCH, 128).transpose(0, 4, 3, 1, 2))
    return m


def assemble(C, res):
    L = C.DEPTH
    y_p = np.zeros((C.BATCH, C.SEQ, C.D), np.float32)
    y_s = np.zeros((C.DEC_BATCH, C.TS, C.D), np.float32)
    ret_p = np.zeros((L, C.BATCH, C.RH, 128, 128), np.float32)
    ret_s = np.zeros((L, C.DEC_BATCH, C.RH, 128, 128), np.float32)
    wk_p = np.zeros((L, C.BATCH, 128, 2, 64), np.float32)
    wk_s = np.zeros((L, C.DEC_BATCH, 128, 2, 64), np.float32)
    wv_p = np.zeros((L, C.BATCH, 128, 2, 64), np.float32)
    wv_s = np.zeros((L, C.DEC_BATCH, 128, 2, 64), np.float32)
    cv_p = np.zeros((L, C.BATCH, 2, C.CC), np.float32)
    cv_s = np.zeros((L, C.DEC_BATCH, 2, C.CC), np.float32)
    for core in range(NCORE):
        r = res[core]
        seq, seg = core // C.SEGS, core % C.SEGS
        b0 = core * C.NSQ
        y = r["yT"].T
        y_p[seq, seg * C.NP:(seg + 1) * C.NP] = y[:C.NP]
        y_s[b0:b0 + C.NSQ] = y[C.NP:].reshape(C.NSQ, C.TS, C.D)
        ret_s[:, b0:b0 + C.NSQ] = r["o_rets"]
        wk_s[:, b0:b0 + C.NSQ] = r["o_wks"].transpose(0, 1, 4, 2, 3)
        wv_s[:, b0:b0 + C.NSQ] = r["o_wvs"].transpose(0, 1, 3, 2).reshape(L, C.NSQ, 128, 2, 64)
        cv_s[:, b0:b0 + C.NSQ] = r["o_cvs"].transpose(0, 3, 4, 2, 1).reshape(L, C.NSQ, 2, C.CC)
        if seg == C.SEGS - 1:
            ret_p[:, seq] = r["o_retp"]
            wk_p[:, seq] = r["o_wkp"].transpose(0, 3, 1, 2)
            wv_p[:, seq] = r["o_wvp"].transpose(0, 2, 1).reshape(L, 128, 2, 64)
            cv_p[:, seq] = r["o_cvp"].transpose(0, 3, 2, 1).reshape(L, 2, C.CC)
    return (y_p, y_s, ret_p, ret_s, wk_p, wk_s, wv_p, wv_s, cv_p, cv_s)


_NC_CACHE = {}


def run_cfg(C, inp, trace=False):
    key = (C.D, C.SEQ, C.DEPTH)
    if key not in _NC_CACHE:
        _NC_CACHE[key] = build(C)
    nc = _NC_CACHE[key]
    inp = {k: np.asarray(v) for k, v in inp.items()}
    sh = prep_shared(C, inp)
    in_maps = [prep_core(C, inp, c, sh) for c in range(NCORE)]
    res = run_bass_kernel_spmd(nc, in_maps, core_ids=list(range(NCORE)), trace=trace)
    return assemble(C, res.results), res


def kernel(**inputs):
    C = Cfg()
    out, _ = run_cfg(C, inputs)
    return out
```
